# Optimizing a Trainium2 kernel written in Bass

```python
import math
import jax
import jax.numpy as jnp
from jax import lax
import numpy as np

D_MODEL = 1024
BATCH = 8
SEQ = 4096
DEPTH = 2

N_A_LAYERS = DEPTH // 2
N_B_LAYERS = DEPTH - N_A_LAYERS
N_DENSE_FFN = (DEPTH + 1) // 2
N_MOE_FFN = DEPTH // 2

D_RNN = D_MODEL
RNN_BLOCKS = 8
RNN_BLOCK_W = D_RNN // RNN_BLOCKS
CONV_W = 4
LRU_C = 8.0

MEM_TOKENS = 256
MEM_HEADS = 4
MEM_HEAD_DIM = D_MODEL // 8
MEM_W = MEM_HEADS * MEM_HEAD_DIM

DIFF_HEADS = 8
DIFF_HEAD_DIM = D_MODEL // 16
DIFF_V_DIM = 2 * DIFF_HEAD_DIM
DIFF_QK_W = DIFF_HEADS * 2 * DIFF_HEAD_DIM
DIFF_V_W = DIFF_HEADS * DIFF_V_DIM
Q_BLOCK = 128

FFN_DIM = (7 * D_MODEL) // 2
N_EXPERTS = 8
TOP_K = 2
EXPERT_DIM = FFN_DIM
MOE_BLOCK = 256

LN_EPS = 1e-5
DEEPNORM_ALPHA = (2.0 * DEPTH) ** 0.25
DEEPNORM_BETA = (8.0 * DEPTH) ** -0.25

kernel_name = "yoco_rglru_diffattn_moe_deepnorm"


def layer_norm(x, g, b):
    xf = x.astype(jnp.float32)
    mu = jnp.mean(xf, -1, keepdims=True)
    var = jnp.mean(jnp.square(xf - mu), -1, keepdims=True)
    return ((xf - mu) * lax.rsqrt(var + LN_EPS) * g + b).astype(x.dtype)


def rms_norm(x, g):
    xf = x.astype(jnp.float32)
    return (xf * lax.rsqrt(jnp.mean(jnp.square(xf), -1, keepdims=True) + LN_EPS) * g).astype(x.dtype)


def memory_attention(q, mem, w_mem_kv):
    B, T, _ = q.shape
    kv = mem @ w_mem_kv
    k = kv[..., :MEM_W].reshape(B, -1, MEM_HEADS, MEM_HEAD_DIM)
    v = kv[..., MEM_W:].reshape(B, -1, MEM_HEADS, MEM_HEAD_DIM)
    qh = q.reshape(B, T, MEM_HEADS, MEM_HEAD_DIM)
    s = jnp.einsum('bthd,bmhd->bhtm', qh, k).astype(jnp.float32) * (MEM_HEAD_DIM ** -0.5)
    p = jax.nn.softmax(s, axis=-1).astype(v.dtype)
    o = jnp.einsum('bhtm,bmhd->bthd', p, v)
    return o.reshape(B, T, MEM_W)


def causal_depthwise_conv(u, w, b):
    out = lax.conv_general_dilated(
        u, w[:, None, :], window_strides=(1,), padding=[(CONV_W - 1, 0)],
        dimension_numbers=('NWC', 'WIO', 'NWC'), feature_group_count=u.shape[-1])
    return out + b


def rg_lru(u, w_r, b_r, w_i, b_i, lam):
    B, T, C = u.shape
    ub = u.reshape(B, T, RNN_BLOCKS, RNN_BLOCK_W)
    r = jax.nn.sigmoid(jnp.einsum('btnc,ncd->btnd', ub, w_r).reshape(B, T, C) + b_r)
    i = jax.nn.sigmoid(jnp.einsum('btnc,ncd->btnd', ub, w_i).reshape(B, T, C) + b_i)
    log_a = -LRU_C * r.astype(jnp.float32) * jax.nn.softplus(-lam.astype(jnp.float32))
    a = jnp.exp(log_a)
    mult = jnp.sqrt(-jnp.expm1(2.0 * log_a))
    mult = jnp.where((jnp.arange(T) == 0)[None, :, None], 1.0, mult)
    bx = mult * (i * u).astype(jnp.float32)

    def combine(left, right):
        a_l, b_l = left
        a_r, b_r2 = right
        return a_r * a_l, a_r * b_l + b_r2

    _, h = lax.associative_scan(combine, (a, bx), axis=1)
    return h.astype(u.dtype)


def recurrent_mixer(h, mem, w_in, conv_w, conv_b, w_r, b_r, w_i, b_i, lam, w_out, w_mem_kv):
    z = h @ w_in
    gate = jax.nn.gelu(z[..., :D_RNN])
    rec = causal_depthwise_conv(z[..., D_RNN:2 * D_RNN], conv_w, conv_b)
    rnn_out = rg_lru(rec, w_r, b_r, w_i, b_i, lam) * gate
    mem_out = memory_attention(z[..., 2 * D_RNN:], mem, w_mem_kv)
    return jnp.concatenate([rnn_out, mem_out], axis=-1) @ w_out


def differential_attention(q, k, v, lam_vecs, subln_g, lam_init):
    B, T = q.shape[0], q.shape[1]
    lv = lam_vecs.astype(jnp.float32)
    lam = jnp.exp(jnp.sum(lv[0] * lv[1])) - jnp.exp(jnp.sum(lv[2] * lv[3])) + lam_init
    slopes = jnp.exp2(-8.0 * (jnp.arange(DIFF_HEADS, dtype=jnp.float32) + 1.0) / DIFF_HEADS)
    pos = jnp.arange(T, dtype=jnp.float32)
    scale = DIFF_HEAD_DIM ** -0.5
    outs = []
    for start in range(0, T, Q_BLOCK):
        end = start + Q_BLOCK
        s = jnp.einsum('bqhcd,bkhcd->bhcqk', q[:, start:end], k[:, :end]).astype(jnp.float32) * scale
        dist = pos[start:end, None] - pos[None, :end]
        s = s - slopes[None, :, None, None, None] * dist
        s = jnp.where(dist >= 0, s, -jnp.inf)
        p = jax.nn.softmax(s, axis=-1)
        wgt = (p[:, :, 0] - lam * p[:, :, 1]).astype(v.dtype)
        outs.append(jnp.einsum('bhqk,bkhe->bqhe', wgt, v[:, :end]))
    o = jnp.concatenate(outs, axis=1)
    o = rms_norm(o, subln_g) * (1.0 - lam_init)
    return o.reshape(B, T, DIFF_V_W)


def diff_mixer(h, mem, k_sh, v_sh, w_q, lam_vecs, subln_g, w_out, w_mem_kv, lam_init):
    B, T, _ = h.shape
    q = h @ w_q
    q_diff = q[..., :DIFF_QK_W].reshape(B, T, DIFF_HEADS, 2, DIFF_HEAD_DIM)
    attn = differential_attention(q_diff, k_sh, v_sh, lam_vecs, subln_g, lam_init)
    mem_out = memory_attention(q[..., DIFF_QK_W:], mem, w_mem_kv)
    return jnp.concatenate([attn, mem_out], axis=-1) @ w_out


def swiglu(h, w13, w2):
    gu = h @ w13
    f = w2.shape[0]
    return (jax.nn.silu(gu[..., :f]) * gu[..., f:]) @ w2


def moe_swiglu(h, w_router, w13, w2):
    B, T, D = h.shape
    xt = h.reshape(-1, D)
    n_tok = xt.shape[0]
    logits = (xt @ w_router).astype(jnp.float32)
    top_v, top_e = lax.top_k(logits, TOP_K)
    gates = jax.nn.softmax(top_v, axis=-1)
    flat_e = top_e.reshape(-1)
    order = jnp.argsort(flat_e)
    sorted_e = flat_e[order]
    sorted_tok = order // TOP_K
    sorted_gate = gates.reshape(-1)[order].astype(xt.dtype)
    counts = jnp.bincount(flat_e, length=N_EXPERTS)
    padded = (counts + MOE_BLOCK - 1) // MOE_BLOCK * MOE_BLOCK
    start = jnp.cumsum(counts) - counts
    pstart = jnp.cumsum(padded) - padded
    dest = pstart[sorted_e] + jnp.arange(n_tok * TOP_K) - start[sorted_e]
    n_rows = n_tok * TOP_K + N_EXPERTS * MOE_BLOCK
    n_blocks = n_rows // MOE_BLOCK
    buf = jnp.zeros((n_rows, D), xt.dtype).at[dest].set(xt[sorted_tok])
    block_e = jnp.searchsorted(jnp.cumsum(padded), jnp.arange(n_blocks) * MOE_BLOCK, side='right')
    block_e = jnp.minimum(block_e, N_EXPERTS - 1)

    def expert_block(args):
        xb, e = args
        gu = xb @ w13[e]
        return (jax.nn.silu(gu[:, :EXPERT_DIM]) * gu[:, EXPERT_DIM:]) @ w2[e]

    yb = lax.map(expert_block, (buf.reshape(n_blocks, MOE_BLOCK, D), block_e))
    y_sorted = yb.reshape(n_rows, D)[dest] * sorted_gate[:, None]
    y = jax.ops.segment_sum(y_sorted, sorted_tok, num_segments=n_tok)
    return y.reshape(B, T, D)


def setup_inputs(seed: int = 0) -> dict:
    key = jax.random.key(seed)
    ks = jax.random.split(key, 32)
    D = D_MODEL

    def nrm(k, shape, scale):
        return jax.random.normal(k, shape, jnp.float32) * scale

    a_pow_c = jax.random.uniform(ks[9], (N_A_LAYERS, D_RNN), jnp.float32, 0.9, 0.999)
    a_base = a_pow_c ** (1.0 / LRU_C)
    a_lambda = jnp.log(a_base) - jnp.log1p(-a_base)
    return {
        "x": nrm(ks[0], (BATCH, SEQ, D), 1.0),
        "mem": nrm(ks[1], (BATCH, MEM_TOKENS, D), 1.0),
        "a_w_in": nrm(ks[2], (N_A_LAYERS, D, 2 * D_RNN + MEM_W), D ** -0.5),
        "a_conv_w": nrm(ks[3], (N_A_LAYERS, CONV_W, D_RNN), CONV_W ** -0.5),
        "a_conv_b": nrm(ks[4], (N_A_LAYERS, D_RNN), 0.01),
        "a_w_rgate": nrm(ks[5], (N_A_LAYERS, RNN_BLOCKS, RNN_BLOCK_W, RNN_BLOCK_W), RNN_BLOCK_W ** -0.5),
        "a_b_rgate": nrm(ks[6], (N_A_LAYERS, D_RNN), 0.1),
        "a_w_igate": nrm(ks[7], (N_A_LAYERS, RNN_BLOCKS, RNN_BLOCK_W, RNN_BLOCK_W), RNN_BLOCK_W ** -0.5),
        "a_b_igate": nrm(ks[8], (N_A_LAYERS, D_RNN), 0.1),
        "a_lambda": a_lambda,
        "a_w_out": nrm(ks[10], (N_A_LAYERS, D_RNN + MEM_W, D), (D_RNN + MEM_W) ** -0.5 * DEEPNORM_BETA),
        "w_kv_shared": nrm(ks[11], (D, DIFF_QK_W + DIFF_V_W), D ** -0.5),
        "b_w_q": nrm(ks[12], (N_B_LAYERS, D, DIFF_QK_W + MEM_W), D ** -0.5),
        "b_lambda": nrm(ks[13], (N_B_LAYERS, 4, DIFF_HEAD_DIM), 0.1),
        "b_subln_g": 1.0 + nrm(ks[14], (N_B_LAYERS, DIFF_V_DIM), 0.02),
        "b_w_out": nrm(ks[15], (N_B_LAYERS, DIFF_V_W + MEM_W, D), (DIFF_V_W + MEM_W) ** -0.5 * DEEPNORM_BETA),
        "mem_w_kv": nrm(ks[16], (DEPTH, D, 2 * MEM_W), D ** -0.5),
        "ffn_w13": nrm(ks[17], (N_DENSE_FFN, D, 2 * FFN_DIM), D ** -0.5),
        "ffn_w2": nrm(ks[18], (N_DENSE_FFN, FFN_DIM, D), FFN_DIM ** -0.5 * DEEPNORM_BETA),
        "moe_router": nrm(ks[19], (N_MOE_FFN, D, N_EXPERTS), D ** -0.5),
        "moe_w13": nrm(ks[20], (N_MOE_FFN, N_EXPERTS, D, 2 * EXPERT_DIM), D ** -0.5),
        "moe_w2": nrm(ks[21], (N_MOE_FFN, N_EXPERTS, EXPERT_DIM, D), EXPERT_DIM ** -0.5 * DEEPNORM_BETA),
        "ln_g": 1.0 + nrm(ks[22], (DEPTH, 2, D), 0.02),
        "ln_b": nrm(ks[23], (DEPTH, 2, D), 0.01),
    }


def reference(x, mem, a_w_in, a_conv_w, a_conv_b, a_w_rgate, a_b_rgate, a_w_igate, a_b_igate,
              a_lambda, a_w_out, w_kv_shared, b_w_q, b_lambda, b_subln_g, b_w_out, mem_w_kv,
              ffn_w13, ffn_w2, moe_router, moe_w13, moe_w2, ln_g, ln_b):
    h = x
    B, T, _ = x.shape
    k_sh = None
    v_sh = None
    for layer in range(DEPTH):
        if layer < N_A_LAYERS:
            i = layer
            y = recurrent_mixer(h, mem, a_w_in[i], a_conv_w[i], a_conv_b[i], a_w_rgate[i], a_b_rgate[i],
                                a_w_igate[i], a_b_igate[i], a_lambda[i], a_w_out[i], mem_w_kv[layer])
        else:
            j = layer - N_A_LAYERS
            if j == 0:
                kv = h @ w_kv_shared
                k_sh = kv[..., :DIFF_QK_W].reshape(B, T, DIFF_HEADS, 2, DIFF_HEAD_DIM)
                v_sh = kv[..., DIFF_QK_W:].reshape(B, T, DIFF_HEADS, DIFF_V_DIM)
            lam_init = 0.8 - 0.6 * math.exp(-0.3 * layer)
            y = diff_mixer(h, mem, k_sh, v_sh, b_w_q[j], b_lambda[j], b_subln_g[j], b_w_out[j],
                           mem_w_kv[layer], lam_init)
        h = layer_norm(DEEPNORM_ALPHA * h + y, ln_g[layer, 0], ln_b[layer, 0])
        if layer % 2 == 0:
            y = swiglu(h, ffn_w13[layer // 2], ffn_w2[layer // 2])
        else:
            y = moe_swiglu(h, moe_router[layer // 2], moe_w13[layer // 2], moe_w2[layer // 2])
        h = layer_norm(DEEPNORM_ALPHA * h + y, ln_g[layer, 1], ln_b[layer, 1])
    return h
```

```python
import math
from contextlib import ExitStack

import numpy as np
import ml_dtypes

import concourse.bass as bass
import concourse.mybir as mybir
from concourse.bass_utils import run_bass_kernel_spmd

F32 = mybir.dt.float32
BF16 = mybir.dt.bfloat16
I32 = mybir.dt.int32
AF = mybir.ActivationFunctionType
ALU = mybir.AluOpType
AX = mybir.AxisListType

T = 4096
D = 1024
NT = T // 128
FFN = 3584
NE = 8
CAP = 1280
ALPHA = (2.0 * 2) ** 0.25
EPS = 1e-5
LAM_INIT = 0.8 - 0.6 * math.exp(-0.3 * 1)
NEG_BIG = -1.0e9
DBG_GMUL = False
DBG_LNEXP = False


class Buf:
    def __init__(self, name, t=None, space="sbuf"):
        self.name = name
        self.t = t
        self.space = space
        self.W = {}
        self.R = {}
        self.ds = None
        self.nowaw = False

    def __getitem__(self, idx):
        return self.t[idx]


def _merge(dst, src):
    for k, (s, v) in src.items():
        if k not in dst or dst[k][1] < v:
            dst[k] = (s, v)


class KB:
    ENGS = ("pe", "act", "dve", "pool", "sp")

    def __init__(self):
        self.nc = bass.Bass("TRN2", target_bir_lowering=False)
        nc = self.nc
        self.eng = {"pe": nc.tensor, "act": nc.scalar, "dve": nc.vector, "pool": nc.gpsimd, "sp": nc.sync}
        self.root = ExitStack()
        self.stacks = [self.root]
        self.sem = {}
        self.cnt = {}
        self.key = {}
        self.epoch = {}
        self.uid = 0
        for e in self.ENGS:
            self.sem[e] = self.root.enter_context(nc.semaphore("s_" + e))
            self.cnt[e] = 0
            self.epoch[e] = 0
            self.key[e] = e + "#0"
        self.waited = {e: {} for e in self.ENGS}
        self.dsems = []
        self.dpool = {"sw": [], "hw": []}
        self.nins = 0
        self.bregs = {}

    def _nm(self, name):
        self.uid += 1
        return f"{name}_{self.uid}"

    def sb(self, name, shape, dtype=F32):
        t = self.stacks[-1].enter_context(self.nc.sbuf_tensor(self._nm(name), list(shape), dtype))
        return Buf(name, t, "sbuf")

    def ps(self, name, shape, dtype=F32):
        t = self.stacks[-1].enter_context(self.nc.psum_tensor(self._nm(name), list(shape), dtype))
        return Buf(name, t, "psum")

    def dram(self, name, shape, dtype, kind=None):
        if kind is None:
            t = self.nc.dram_tensor(name, list(shape), dtype)
        else:
            t = self.nc.dram_tensor(name, list(shape), dtype, kind=kind)
        b = Buf(name, t, "dram")
        b.ap = t.ap()
        return b

    SEM_LIMIT = 30000

    def _dsem(self, b, kind):
        if b.ds is None:
            b.ds = {}
        if kind not in b.ds:
            pool = self.dpool[kind]
            pool.sort(key=lambda x: x[2])
            if pool and pool[0][2] * 16 < self.SEM_LIMIT:
                ent = list(pool.pop(0))
            else:
                sem = self.root.enter_context(self.nc.semaphore(self._nm("d%s_%s" % (kind, b.name))))
                ent = ["d%s%d" % (kind, self.uid), sem, 0]
            b.ds[kind] = ent
            self.dsems.append((b, kind))
        return b.ds[kind]

    def _wait(self, eng, need):
        e = self.eng[eng]
        w = self.waited[eng]
        for k, (s, v) in need.items():
            if eng == "pe" and k.startswith("pe#"):
                continue
            if w.get(k, 0) < v:
                e.wait_ge(s, v)
                w[k] = v

    def _deps(self, reads, writes):
        need = {}
        for b in reads:
            _merge(need, b.W)
        for b in writes:
            if not b.nowaw:
                _merge(need, b.W)
            _merge(need, b.R)
        return need

    def _commit(self, key, tok, reads, writes):
        for b in reads:
            if key not in b.R or b.R[key][1] < tok[1]:
                b.R[key] = tok
        for b in writes:
            if b.nowaw:
                if key not in b.W or b.W[key][1] < tok[1]:
                    b.W[key] = tok
            else:
                b.W = {key: tok}
            b.R = {}

    def op(self, eng, fn, reads=(), writes=(), inc=True):
        self._wait(eng, self._deps(reads, writes))
        if inc and self.cnt[eng] >= self.SEM_LIMIT:
            self.epoch[eng] += 1
            self.sem[eng] = self.root.enter_context(self.nc.semaphore(self._nm("s_" + eng)))
            self.cnt[eng] = 0
            self.key[eng] = "%s#%d" % (eng, self.epoch[eng])
        ins = fn(self.eng[eng])
        self.nins += 1
        if inc:
            self.cnt[eng] += 1
            ins.then_inc(self.sem[eng], 1)
            tok = (self.sem[eng], self.cnt[eng])
        else:
            tok = (self.sem[eng], self.cnt[eng] + 1)
        self._commit(self.key[eng], tok, reads, writes)
        return ins

    def dma(self, eng, out, in_, reads=(), writes=(), semb=None, **kw):
        self._wait(eng, self._deps(reads, writes))
        if semb is None:
            cand = [b for b in list(writes) + list(reads) if b.space == "sbuf"]
            semb = cand[0] if cand else list(writes)[0]
        ent = self._dsem(semb, "sw" if eng == "pool" else "hw")
        ins = self.eng[eng].dma_start(out=out, in_=in_, **kw)
        ent[2] += 1
        ins.then_inc(ent[1], 16)
        self.nins += 1
        self._commit(ent[0], (ent[1], 16 * ent[2]), reads, writes)
        return ins

    def idma(self, out, out_off, in_, in_off, bound, reads=(), writes=(), semb=None):
        self._wait("pool", self._deps(reads, writes))
        ent = self._dsem(semb, "sw")
        if bound not in self.bregs:
            r = self.nc.gpsimd.alloc_register("bnd%d" % bound)
            self.nc.gpsimd.reg_mov(r, bound)
            self.bregs[bound] = r
        ins = self.nc.gpsimd.indirect_dma_start(out=out, out_offset=out_off, in_=in_, in_offset=in_off,
                                                bounds_check=self.bregs[bound], oob_is_err=False)
        ent[2] += 1
        ins.then_inc(ent[1], 16)
        self.nins += 1
        self._commit(ent[0], (ent[1], 16 * ent[2]), reads, writes)
        return ins

    def barrier(self):
        need = {}
        for e in self.ENGS:
            if self.cnt[e] > 0:
                need[self.key[e]] = (self.sem[e], self.cnt[e])
        for (b, kind) in self.dsems:
            ent = b.ds[kind]
            if ent[2] > 0:
                need[ent[0]] = (ent[1], 16 * ent[2])
        for e in self.ENGS:
            n2 = {k: v for k, v in need.items() if k != self.key[e]}
            self._wait(e, n2)

    class _Scope:
        def __init__(self, kb):
            self.kb = kb

        def __enter__(self):
            self.kb.stacks.append(ExitStack())
            self.ndsem = len(self.kb.dsems)
            return self

        def __exit__(self, *a):
            self.kb.barrier()
            st = self.kb.stacks.pop()
            for (b, kind) in self.kb.dsems[self.ndsem:]:
                self.kb.dpool[kind].append(tuple(b.ds[kind]))
                del b.ds[kind]
            del self.kb.dsems[self.ndsem:]
            st.close()
            return False

    def scope(self):
        return KB._Scope(self)

    def finish(self):
        self.barrier()
        self.root.close()


def load_cast(kb, dst, dst_ap, src_buf, src_ap):
    kb.dma("pool", dst_ap, src_ap, reads=[src_buf], writes=[dst])


def load(kb, dst, dst_ap, src_buf, src_ap):
    kb.dma("sp", dst_ap, src_ap, reads=[src_buf], writes=[dst])


def tm_to_fm(kb, src, dstT, col0, ident, ptr, nk=8, ev="act"):
    for k in range(nk):
        kb.op("pe", lambda e, k=k: e.transpose(out=ptr[:, k * 128:(k + 1) * 128], in_=src[:, k * 128:(k + 1) * 128],
                                               identity=ident[:]),
              reads=[src, ident], writes=[ptr], inc=(k == nk - 1))
    pv = ptr[:, 0:nk * 128].rearrange("p (k t) -> p k t", k=nk)
    if ev == "act":
        kb.op("act", lambda e: e.copy(out=dstT[:, 0:nk, col0:col0 + 128], in_=pv), reads=[ptr], writes=[dstT])
    else:
        kb.op("dve", lambda e: e.tensor_copy(out=dstT[:, 0:nk, col0:col0 + 128], in_=pv), reads=[ptr], writes=[dstT])


def mm_group(kb, out_ps, out_ap, pairs, reads):
    n = len(pairs)
    for i, (l, r) in enumerate(pairs):
        kb.op("pe", lambda e, l=l, r=r, i=i: e.matmul(out_ap, l, r, start=(i == 0), stop=(i == n - 1)),
              reads=reads, writes=[out_ps], inc=(i == n - 1))


def host_consts():
    c = {}
    c["ident"] = np.eye(128, dtype=np.float32).astype(ml_dtypes.bfloat16)
    c["ones_bf"] = np.ones((128, 128), dtype=np.float32).astype(ml_dtypes.bfloat16)
    ki = np.arange(128)[:, None].astype(np.float64)
    qi = np.arange(512)[None, :].astype(np.float64)
    bt = np.zeros((5, 128, 512), np.float32)
    bt[0] = -(qi - ki)
    for j in range(4):
        d = qi - ki - 128 * j
        bt[1 + j] = np.where(d >= 0, -(qi - ki), NEG_BIG)
    c["btiles"] = np.ascontiguousarray(bt.transpose(1, 0, 2))
    tri = (np.arange(128)[:, None] < np.arange(128)[None, :]).astype(np.float32)
    c["tri"] = tri
    c["ones_f"] = np.ones((128, 128), np.float32)
    c["eoff"] = np.tile((np.arange(NE) * CAP).astype(np.float32)[None, :], (128, 1))
    return c


def pack_cols(v, nchunk):
    return np.ascontiguousarray(np.asarray(v, np.float32).reshape(nchunk, 128).T)


def mem_kv_prep(kb, memT, wkv, kT, vM, psA):
    for h in range(4):
        mm_group(kb, psA, psA[:, 0:256], [(wkv[:, k, h * 128:(h + 1) * 128], memT[:, k, :]) for k in range(8)],
                 reads=[wkv, memT])
        kb.op("act", lambda e, h=h: e.copy(out=kT[:, h, :], in_=psA[:, 0:256]), reads=[psA], writes=[kT])
    for mc in range(2):
        mm_group(kb, psA, psA[:, 0:512], [(memT[:, k, mc * 128:(mc + 1) * 128], wkv[:, k, 512:1024]) for k in range(8)],
                 reads=[wkv, memT])
        kb.op("act", lambda e, mc=mc: e.copy(out=vM[:, mc, :], in_=psA[:, 0:512]), reads=[psA], writes=[vM])


def mem_attn_tile(kb, h, qps, kT, vM, ones_bf, mq, pT, rs, ob, psS, psO, psR, cat_dram, chunk, t0):
    sc = 128 ** -0.5
    kb.op("act", lambda e: e.copy(out=mq[:], in_=qps[:, 0:512]), reads=[qps], writes=[mq])
    for mc in range(2):
        ps = psS[mc]
        mm_group(kb, ps, ps[:, 0:512], [(kT[:, h, mc * 128:(mc + 1) * 128], mq[:])], reads=[kT, mq])
        kb.op("act", lambda e, mc=mc, ps=ps: e.activation(out=pT[:, mc, :], in_=ps[:, 0:512], func=AF.Exp, scale=sc),
              reads=[ps], writes=[pT])
    mm_group(kb, psR, psR[:, 0:512], [(ones_bf[:], pT[:, mc, :]) for mc in range(2)], reads=[ones_bf, pT])
    mm_group(kb, psO, psO[:, 0:512], [(vM[:, mc, h * 128:(h + 1) * 128], pT[:, mc, :]) for mc in range(2)],
             reads=[vM, pT])
    kb.op("dve", lambda e: e.reciprocal(out=rs[:], in_=psR[:, 0:512]), reads=[psR], writes=[rs])
    kb.op("dve", lambda e: e.tensor_tensor(out=ob[:], in0=psO[:, 0:512], in1=rs[:], op=ALU.mult),
          reads=[psO, rs], writes=[ob])
    kb.dma("sp", cat_dram.ap[chunk, :, t0:t0 + 512], ob[:], reads=[ob], writes=[cat_dram])


def phase1(kb, x, mem, w_in, w_r, w_i, wkv0, pp1, ident_d, ones_d, catT):
    QN = 1024
    with kb.scope():
        ident = kb.sb("ident", [128, 128], BF16)
        ones_bf = kb.sb("ones", [128, 128], BF16)
        load(kb, ident, ident[:], ident_d, ident_d.ap)
        load(kb, ones_bf, ones_bf[:], ones_d, ones_d.ap)
        pp = kb.sb("pp", [128, 80], F32)
        load(kb, pp, pp[:], pp1, pp1.ap)
        xT = kb.sb("xT", [128, 8, T], BF16)
        wi = kb.sb("wi", [128, 8, 2560], BF16)
        w_in_v = w_in.ap.rearrange("(k p) n -> p k n", p=128)
        for j in range(5):
            load_cast(kb, wi, wi[:, :, j * 512:(j + 1) * 512], w_in, w_in_v[:, :, j * 512:(j + 1) * 512])
        wr = kb.sb("wr", [128, 8, 128], BF16)
        wg = kb.sb("wg", [128, 8, 128], BF16)
        load_cast(kb, wr, wr[:], w_r, w_r.ap.rearrange("n c d -> c n d"))
        load_cast(kb, wg, wg[:], w_i, w_i.ap.rearrange("n c d -> c n d"))
        wkv = kb.sb("wkv", [128, 8, 1024], BF16)
        load_cast(kb, wkv, wkv[:], wkv0, wkv0.ap.rearrange("(k p) n -> p k n", p=128))
        memT = kb.sb("memT", [128, 8, 256], BF16)
        kT = kb.sb("kT", [128, 4, 256], BF16)
        vM = kb.sb("vM", [128, 2, 512], BF16)
        xb = [kb.sb("xb%d" % i, [128, 1024], BF16) for i in range(2)]
        ptr = [kb.ps("ptr%d" % i, [128, 1024], BF16) for i in range(2)]
        psA = [kb.ps("psA%d" % i, [128, 512], F32) for i in range(6)]
        LAM, NSP, TMP = 56, 64, 72
        kb.op("act", lambda e: e.activation(out=pp[:, TMP:TMP + 8], in_=pp[:, LAM:LAM + 8], func=AF.Exp, scale=-1.0),
              reads=[pp], writes=[pp])
        kb.op("dve", lambda e: e.tensor_scalar(out=pp[:, NSP:NSP + 8], in0=pp[:, TMP:TMP + 8], scalar1=-0.25,
                                               scalar2=1.0 / 3.0, op0=ALU.mult, op1=ALU.add), reads=[pp], writes=[pp])
        kb.op("dve", lambda e: e.tensor_tensor(out=pp[:, NSP:NSP + 8], in0=pp[:, NSP:NSP + 8], in1=pp[:, TMP:TMP + 8],
                                               op=ALU.mult), reads=[pp], writes=[pp])
        kb.op("dve", lambda e: e.tensor_scalar(out=pp[:, NSP:NSP + 8], in0=pp[:, NSP:NSP + 8], scalar1=-1.0,
                                               scalar2=0.5, op0=ALU.mult, op1=ALU.add), reads=[pp], writes=[pp])
        kb.op("dve", lambda e: e.tensor_tensor(out=pp[:, NSP:NSP + 8], in0=pp[:, NSP:NSP + 8], in1=pp[:, TMP:TMP + 8],
                                               op=ALU.mult), reads=[pp], writes=[pp])
        kb.op("dve", lambda e: e.tensor_scalar(out=pp[:, NSP:NSP + 8], in0=pp[:, NSP:NSP + 8], scalar1=-1.0,
                                               scalar2=1.0, op0=ALU.mult, op1=ALU.add), reads=[pp], writes=[pp])
        kb.op("dve", lambda e: e.tensor_tensor(out=pp[:, NSP:NSP + 8], in0=pp[:, NSP:NSP + 8], in1=pp[:, TMP:TMP + 8],
                                               op=ALU.mult), reads=[pp], writes=[pp])
        kb.op("dve", lambda e: e.tensor_scalar(out=pp[:, NSP:NSP + 8], in0=pp[:, NSP:NSP + 8], scalar1=-8.0,
                                               scalar2=None, op0=ALU.mult), reads=[pp], writes=[pp])
        for tc in range(NT):
            b_ = xb[tc % 2]
            load_cast(kb, b_, b_[:], x, x.ap[tc * 128:(tc + 1) * 128, :])
            tm_to_fm(kb, b_, xT, tc * 128, ident, ptr[tc % 2], ev=("act" if tc % 2 == 0 else "dve"))
        for mc in range(2):
            b_ = xb[mc % 2]
            load_cast(kb, b_, b_[:], mem, mem.ap[mc * 128:(mc + 1) * 128, :])
            tm_to_fm(kb, b_, memT, mc * 128, ident, ptr[mc % 2])
        mem_kv_prep(kb, memT, wkv, kT, vM, psA[0])
        mq = kb.sb("mq", [128, 512], BF16)
        pT = kb.sb("pT", [128, 2, 512], BF16)
        rs = kb.sb("rs", [128, 512], F32)
        obm = [kb.sb("obm%d" % i, [128, 512], BF16) for i in range(2)]
        it = 0
        for tt in range(T // 512):
            for h in range(4):
                mm_group(kb, psA[0], psA[0][:, 0:512],
                         [(wi[:, k, 2048 + h * 128:2048 + (h + 1) * 128], xT[:, k, tt * 512:(tt + 1) * 512]) for k in range(8)],
                         reads=[wi, xT])
                mem_attn_tile(kb, h, psA[0], kT, vM, ones_bf, mq, pT, rs, obm[it % 2], [psA[1], psA[2]], psA[3], psA[4],
                              catT, 8 + h, tt * 512)
                it += 1
        REC = kb.sb("REC", [128, 3 + QN], F32)
        U = kb.sb("U", [128, QN], F32)
        UB = kb.sb("UB", [128, QN], BF16)
        GT = kb.sb("GT", [128, QN], BF16)
        RG = kb.sb("RG", [128, QN], F32)
        IG = kb.sb("IG", [128, QN], F32)
        A = kb.sb("A", [128, QN], F32)
        M = kb.sb("M", [128, QN], F32)
        H = kb.sb("H", [128, QN], F32)
        hlast = kb.sb("hlast", [128, 1], F32)
        OB = [kb.sb("OB%d" % i, [128, QN], BF16) for i in range(2)]
        it = 0
        for c in range(8):
            kb.op("dve", lambda e: e.memset(REC[:, 0:3], 0.0), writes=[REC])
            for qd in range(T // QN):
                t0 = qd * QN
                for s in range(2):
                    tsl = slice(t0 + s * 512, t0 + (s + 1) * 512)
                    mm_group(kb, psA[s], psA[s][:, 0:512],
                             [(wi[:, k, c * 128:(c + 1) * 128], xT[:, k, tsl]) for k in range(8)], reads=[wi, xT])
                    kb.op("act", lambda e, s=s: e.activation(out=GT[:, s * 512:(s + 1) * 512], in_=psA[s][:, 0:512],
                                                              func=AF.Gelu), reads=[psA[s]], writes=[GT])
                for s in range(2):
                    tsl = slice(t0 + s * 512, t0 + (s + 1) * 512)
                    mm_group(kb, psA[2 + s], psA[2 + s][:, 0:512],
                             [(wi[:, k, 1024 + c * 128:1024 + (c + 1) * 128], xT[:, k, tsl]) for k in range(8)],
                             reads=[wi, xT])
                    kb.op("act", lambda e, s=s: e.copy(out=REC[:, 3 + s * 512:3 + (s + 1) * 512], in_=psA[2 + s][:, 0:512]),
                          reads=[psA[2 + s]], writes=[REC])
                kb.op("dve", lambda e: e.tensor_scalar(out=U[:], in0=REC[:, 3:3 + QN], scalar1=pp[:, c * 4 + 3:c * 4 + 4],
                                                       scalar2=pp[:, 32 + c:33 + c], op0=ALU.mult, op1=ALU.add),
                      reads=[REC, pp], writes=[U])
                for j in range(3):
                    kb.op("dve", lambda e, j=j: e.scalar_tensor_tensor(out=U[:], in0=REC[:, j:j + QN],
                                                                       scalar=pp[:, c * 4 + j:c * 4 + j + 1], in1=U[:],
                                                                       op0=ALU.mult, op1=ALU.add),
                          reads=[REC, pp, U], writes=[U])
                kb.op("pool", lambda e: e.tensor_copy(out=REC[:, 0:3], in_=REC[:, QN:QN + 3]), reads=[REC], writes=[REC])
                kb.op("act", lambda e: e.copy(out=UB[:], in_=U[:]), reads=[U], writes=[UB])
                for s in range(2):
                    mm_group(kb, psA[4], psA[4][:, 0:512], [(wr[:, c, :], UB[:, s * 512:(s + 1) * 512])], reads=[wr, UB])
                    kb.op("act", lambda e, s=s: e.activation(out=RG[:, s * 512:(s + 1) * 512], in_=psA[4][:, 0:512],
                                                              func=AF.Sigmoid, bias=pp[:, 40 + c:41 + c]),
                          reads=[psA[4], pp], writes=[RG])
                    mm_group(kb, psA[5], psA[5][:, 0:512], [(wg[:, c, :], UB[:, s * 512:(s + 1) * 512])], reads=[wg, UB])
                    kb.op("act", lambda e, s=s: e.activation(out=IG[:, s * 512:(s + 1) * 512], in_=psA[5][:, 0:512],
                                                              func=AF.Sigmoid, bias=pp[:, 48 + c:49 + c]),
                          reads=[psA[5], pp], writes=[IG])
                kb.op("act", lambda e: e.activation(out=A[:], in_=RG[:], func=AF.Exp, scale=pp[:, NSP + c:NSP + c + 1]),
                      reads=[RG, pp], writes=[A])
                kb.op("pool", lambda e: e.tensor_tensor(out=M[:], in0=A[:], in1=A[:], op=ALU.mult), reads=[A], writes=[M])
                kb.op("act", lambda e: e.activation(out=M[:], in_=M[:], func=AF.Sqrt, scale=-1.0, bias=1.0),
                      reads=[M], writes=[M])
                if qd == 0:
                    kb.op("dve", lambda e: e.memset(M[:, 0:1], 1.0), writes=[M])
                kb.op("pool", lambda e: e.tensor_tensor(out=IG[:], in0=IG[:], in1=U[:], op=ALU.mult), reads=[IG, U],
                      writes=[IG])
                kb.op("dve", lambda e: e.tensor_tensor(out=M[:], in0=M[:], in1=IG[:], op=ALU.mult), reads=[M, IG],
                      writes=[M])
                if qd == 0:
                    kb.op("dve", lambda e: e.tensor_tensor_scan(out=H[:], data0=A[:], data1=M[:], initial=0.0,
                                                                op0=ALU.mult, op1=ALU.add), reads=[A, M], writes=[H])
                else:
                    kb.op("dve", lambda e: e.tensor_tensor_scan(out=H[:], data0=A[:], data1=M[:], initial=hlast[:, 0:1],
                                                                op0=ALU.mult, op1=ALU.add), reads=[A, M, hlast],
                          writes=[H])
                kb.op("pool", lambda e: e.tensor_copy(out=hlast[:], in_=H[:, QN - 1:QN]), reads=[H], writes=[hlast])
                ob = OB[it % 2]
                it += 1
                kb.op("dve", lambda e, ob=ob: e.tensor_tensor(out=ob[:], in0=H[:], in1=GT[:], op=ALU.mult),
                      reads=[H, GT], writes=[ob])
                kb.dma("sp", catT.ap[c, :, t0:t0 + QN], ob[:], reads=[ob], writes=[catT])


class LNPipe:
    def __init__(self, kb, ln_g, ln_b, ident_d, resid, out_h, hooks=None, transposes=True, gmul="pool", lnexp=False):
        self.kb = kb
        self.gmul, self.lnexp = gmul, lnexp
        self.resid, self.out_h = resid, out_h
        self.hooks = hooks or []
        self.hq = []
        self.gbc = kb.sb("gbc", [128, D], F32)
        self.bbc = kb.sb("bbc", [128, D], F32)
        load(kb, self.gbc, self.gbc[:], ln_g, ln_g.ap.partition_broadcast(128))
        load(kb, self.bbc, self.bbc[:], ln_b, ln_b.ap.partition_broadcast(128))
        self.hres = [kb.sb("hres%d" % i, [128, D], F32) for i in range(2)]
        self.r = [kb.sb("lnr%d" % i, [128, D], F32) for i in range(2)]
        self.xn = [kb.sb("lnx%d" % i, [128, D], F32) for i in range(2)]
        self.st = [kb.sb("lnst%d" % i, [128, 2, 6], F32) for i in range(2)]
        self.mv = [kb.sb("lnmv%d" % i, [128, 4], F32) for i in range(2)]
        self.transposes = transposes
        if transposes:
            self.hb = [kb.sb("lnhb%d" % i, [128, D], BF16) for i in range(2)]
            self.ident = kb.sb("ident", [128, 128], BF16)
            load(kb, self.ident, self.ident[:], ident_d, ident_d.ap)
            self.ptr = kb.ps("lnptr", [128, 1024], BF16)
        self.n_pref = 0
        self.n_push = 0
        self.pend_b = None

    def prefetch(self, row0):
        h = self.hres[self.n_pref % 2]
        self.n_pref += 1
        load(self.kb, h, h[:], self.resid, self.resid.ap[row0:row0 + 128, :])

    def push(self, ysrc, row0, hT_tile=None, hT_col=0, after_c=None):
        kb = self.kb
        i = self.n_push
        self.n_push += 1
        hres, r, st, mv = self.hres[i % 2], self.r[i % 2], self.st[i % 2], self.mv[i % 2]
        for j in range(2):
            kb.op("dve", lambda e, j=j: e.scalar_tensor_tensor(out=r[:, j * 512:(j + 1) * 512], in0=hres[:, j * 512:(j + 1) * 512],
                                                               scalar=ALPHA, in1=ysrc[j][1], op0=ALU.mult, op1=ALU.add),
                  reads=[hres, ysrc[j][0]], writes=[r])
        for j in range(2):
            kb.op("dve", lambda e, j=j: e.bn_stats(out=st[:, j, :], in_=r[:, j * 512:(j + 1) * 512]), reads=[r], writes=[st])
        kb.op("dve", lambda e: e.bn_aggr(out=mv[:, 0:2], in_=st[:]), reads=[st], writes=[mv])
        if self.lnexp:
            kb.op("dve", lambda e: e.tensor_scalar(out=mv[:, 2:3], in0=mv[:, 1:2], scalar1=EPS, scalar2=None, op0=ALU.add), reads=[mv],
                  writes=[mv])
            kb.op("act", lambda e: e.activation(out=mv[:, 2:3], in_=mv[:, 2:3], func=AF.Ln), reads=[mv], writes=[mv])
            kb.op("act", lambda e: e.activation(out=mv[:, 2:3], in_=mv[:, 2:3], func=AF.Exp, scale=-0.5), reads=[mv], writes=[mv])
        else:
            kb.op("act", lambda e: e.activation(out=mv[:, 2:3], in_=mv[:, 1:2], func=AF.Sqrt, bias=EPS, scale=1.0), reads=[mv], writes=[mv])
            kb.op("dve", lambda e: e.reciprocal(out=mv[:, 2:3], in_=mv[:, 2:3]), reads=[mv], writes=[mv])
        kb.op("dve", lambda e: e.tensor_scalar(out=mv[:, 3:4], in0=mv[:, 0:1], scalar1=mv[:, 2:3], scalar2=-1.0, op0=ALU.mult,
                                               op1=ALU.mult), reads=[mv], writes=[mv])
        prev = self.pend_b
        self.pend_b = (i, row0, hT_tile, hT_col, after_c)
        self._run_hooks()
        if prev is not None:
            self._stage_bc(*prev)

    def _run_hooks(self):
        nq = []
        for (ci, row0, st) in self.hq:
            self.hooks[st](self.xn[ci % 2], row0, ci)
            if st + 1 < len(self.hooks):
                nq.append((ci, row0, st + 1))
        self.hq = nq

    def _stage_bc(self, i, row0, hT_tile, hT_col, after_c):
        kb = self.kb
        r, mv, xn = self.r[i % 2], self.mv[i % 2], self.xn[i % 2]
        kb.op("act", lambda e: e.activation(out=xn[:], in_=r[:], func=AF.Identity, scale=mv[:, 2:3], bias=mv[:, 3:4]),
              reads=[r, mv], writes=[xn])
        kb.op(self.gmul, lambda e: e.tensor_tensor(out=xn[:], in0=xn[:], in1=self.gbc[:], op=ALU.mult), reads=[xn, self.gbc], writes=[xn])
        kb.op("dve", lambda e: e.tensor_tensor(out=xn[:], in0=xn[:], in1=self.bbc[:], op=ALU.add), reads=[xn, self.bbc], writes=[xn])
        kb.dma("sp", self.out_h.ap[row0:row0 + 128, :], xn[:], reads=[xn], writes=[self.out_h])
        if self.hooks:
            self.hq.append((i, row0, 0))
        if hT_tile is not None:
            hb = self.hb[i % 2]
            kb.op("act", lambda e: e.copy(out=hb[:], in_=xn[:]), reads=[xn], writes=[hb])
            tm_to_fm(kb, hb, hT_tile, hT_col, self.ident, self.ptr, ev="dve")
        if after_c is not None:
            after_c()

    def flush(self):
        self._run_hooks()
        if self.pend_b is not None:
            self._stage_bc(*self.pend_b)
            self.pend_b = None
        while self.hq:
            self._run_hooks()


def phase_outproj(kb, catT, w_out, resid, ln_g, ln_b, ident_d, out_h, out_hT, hooks_factory=None):
    with kb.scope():
        hooks = hooks_factory(kb) if hooks_factory is not None else None
        P = LNPipe(kb, ln_g, ln_b, ident_d, resid, out_h, hooks=hooks, transposes=(out_hT is not None),
                   gmul=("dve" if hooks and DBG_GMUL else "pool"), lnexp=bool(hooks) and DBG_LNEXP)
        wo = kb.sb("wo", [128, 12, D], BF16)
        wv = w_out.ap.rearrange("(k p) n -> p k n", p=128)
        for j in range(3):
            load_cast(kb, wo, wo[:, j * 4:(j + 1) * 4, :], w_out, wv[:, j * 4:(j + 1) * 4, :])
        ct = [kb.sb("ct%d" % i, [128, 12, 512], BF16) for i in range(2)]
        hTt = [kb.sb("hTt%d" % i, [128, 8, 512], BF16) for i in range(2)] if out_hT is not None else None
        psY = [[kb.ps("psY%d%d" % (a, j), [128, 512], F32) for j in range(2)] for a in range(2)]
        cv = catT.ap.rearrange("c p t -> p c t")
        ov = out_hT.ap.rearrange("k p t -> p k t") if out_hT is not None else None
        NTT = T // 512
        load(kb, ct[0], ct[0][:], catT, cv[:, :, 0:512])
        P.prefetch(0)

        def mm_chunk(i):
            tt, sub = divmod(i, 4)
            c_ = ct[tt % 2]
            if sub == 0 and tt + 1 < NTT:
                n_ = ct[(tt + 1) % 2]
                load(kb, n_, n_[:], catT, cv[:, :, (tt + 1) * 512:(tt + 2) * 512])
            py = psY[i % 2]
            for j in range(2):
                mm_group(kb, py[j], py[j][:, 0:512],
                         [(c_[:, k, sub * 128:(sub + 1) * 128], wo[:, k, j * 512:(j + 1) * 512]) for k in range(12)],
                         reads=[c_, wo])
        mm_chunk(0)
        for i in range(NT):
            tt, sub = divmod(i, 4)
            if i + 1 < NT:
                mm_chunk(i + 1)
                P.prefetch((i + 1) * 128)
            py = psY[i % 2]
            ht = hTt[tt % 2] if hTt is not None else None
            after = None
            if ht is not None and sub == 3:
                def after(ht=ht, tt=tt):
                    kb.dma("sp", ov[:, :, tt * 512:(tt + 1) * 512], ht[:], reads=[ht], writes=[out_hT])
            P.push([(py[0], py[0][:, 0:512]), (py[1], py[1][:, 0:512])], i * 128, ht, sub * 128, after_c=after)
        P.flush()


class FFNState:
    def __init__(self, kb, ntok):
        self.wb = [kb.sb("w13b%d" % i, [128, 8, 512], BF16) for i in range(3)]
        self.sg = [kb.sb("sg%d" % i, [128, 512], BF16) for i in range(2)]
        self.psG = [kb.ps("psG%d" % i, [128, 512], F32) for i in range(2)]
        self.psU = [kb.ps("psU%d" % i, [128, 512], F32) for i in range(2)]
        self.act = kb.sb("actT", [128, 28, ntok], BF16)
        self.nblk = 0
        self.it = 0


def w13_load(kb, FS, w13, w13v, j):
    wb = FS.wb[FS.nblk % 3]
    FS.nblk += 1
    load_cast(kb, wb, wb[:, :, 0:256], w13, w13v[:, :, j * 256:(j + 1) * 256])
    load_cast(kb, wb, wb[:, :, 256:512], w13, w13v[:, :, FFN + j * 256:FFN + (j + 1) * 256])
    return wb


def swiglu_stage1(kb, FS, xs, ntok, w13, w13v, first_blocks=None, next_prefetch=None):
    tiles = []
    t0 = 0
    while t0 < ntok:
        n = min(512, ntok - t0)
        tiles.append((t0, n))
        t0 += n
    blocks = list(first_blocks) if first_blocks else []
    while len(blocks) < 2:
        blocks.append(w13_load(kb, FS, w13, w13v, len(blocks)))
    for j in range(14):
        wb = blocks[j]
        if j + 2 < 14:
            blocks.append(w13_load(kb, FS, w13, w13v, j + 2))
        elif next_prefetch is not None and j + 2 == 15:
            next_prefetch()
        for fc in range(2):
            f = j * 2 + fc
            for (t0, n) in tiles:
                i = FS.it % 2
                FS.it += 1
                pg, pu, sg = FS.psG[i], FS.psU[i], FS.sg[i]
                mm_group(kb, pg, pg[:, 0:n], [(wb[:, k, fc * 128:(fc + 1) * 128], xs[:, k, t0:t0 + n]) for k in range(8)],
                         reads=[wb, xs])
                mm_group(kb, pu, pu[:, 0:n], [(wb[:, k, 256 + fc * 128:256 + (fc + 1) * 128], xs[:, k, t0:t0 + n]) for k in range(8)],
                         reads=[wb, xs])
                kb.op("act", lambda e, pg=pg, sg=sg, n=n: e.activation(out=sg[:, 0:n], in_=pg[:, 0:n], func=AF.Silu),
                      reads=[pg], writes=[sg])
                kb.op("dve", lambda e, pu=pu, sg=sg, n=n, f=f, t0=t0: e.tensor_tensor(out=FS.act[:, f, t0:t0 + n], in0=sg[:, 0:n],
                                                                                   in1=pu[:, 0:n], op=ALU.mult),
                      reads=[sg, pu], writes=[FS.act])


def phase_ffn(kb, hT, resid, w13, w2, ln_g, ln_b, ident_d, out_h, out_hT):
    TS = 1024
    with kb.scope():
        P = LNPipe(kb, ln_g, ln_b, ident_d, resid, out_h)
        FS = FFNState(kb, TS)
        w2s = kb.sb("w2s", [128, 28, D], BF16)
        w2v = w2.ap.rearrange("(k p) n -> p k n", p=128)
        w13v = w13.ap.rearrange("(k p) n -> p k n", p=128)
        xs = kb.sb("xs", [128, 8, TS], BF16)
        hTt = [kb.sb("hTt%d" % i, [128, 8, 512], BF16) for i in range(2)]
        psY = [[FS.psG[a], FS.psU[a]] for a in range(2)]
        hv = hT.ap.rearrange("k p t -> p k t")
        ov = out_hT.ap.rearrange("k p t -> p k t")
        NST = T // TS
        pre = None
        ci = 0
        for st in range(NST):
            load(kb, xs, xs[:], hT, hv[:, :, st * TS:(st + 1) * TS])
            if st == 0:
                pre = [w13_load(kb, FS, w13, w13v, 0)]
                for j in range(7):
                    load_cast(kb, w2s, w2s[:, j * 4:(j + 1) * 4, :], w2, w2v[:, j * 4:(j + 1) * 4, :])
                P.prefetch(0)
            nxt = []

            def prefetch(st=st, nxt=nxt):
                if st + 1 < NST:
                    nxt.append(w13_load(kb, FS, w13, w13v, 0))
            swiglu_stage1(kb, FS, xs, TS, w13, w13v, first_blocks=pre, next_prefetch=prefetch)
            pre = nxt if nxt else None
            def mm2(sub):
                py = psY[sub % 2]
                for j in range(2):
                    mm_group(kb, py[j], py[j][:, 0:512],
                             [(FS.act[:, f, sub * 128:(sub + 1) * 128], w2s[:, f, j * 512:(j + 1) * 512]) for f in range(28)],
                             reads=[FS.act, w2s])
            NSUB = TS // 128
            mm2(0)
            for sub in range(NSUB):
                if sub + 1 < NSUB:
                    mm2(sub + 1)
                py = psY[sub % 2]
                row0 = st * TS + sub * 128
                if row0 + 128 < T:
                    P.prefetch(row0 + 128)
                ht = hTt[(row0 // 512) % 2]
                after = None
                if sub % 4 == 3:
                    def after(ht=ht, c0=(row0 // 512) * 512):
                        kb.dma("sp", ov[:, :, c0:c0 + 512], ht[:], reads=[ht], writes=[out_hT])
                P.push([(py[0], py[0][:, 0:512]), (py[1], py[1][:, 0:512])], row0, ht, (sub % 4) * 128, after_c=after)
        P.flush()


def phase_qkv(kb, hT, mem, w_kvs, w_q, wkv1, ident_d, ones_d, KT, V, QT, catT, xbuf=None):
    with kb.scope():
        ident = kb.sb("ident", [128, 128], BF16)
        ones_bf = kb.sb("ones", [128, 128], BF16)
        load(kb, ident, ident[:], ident_d, ident_d.ap)
        load(kb, ones_bf, ones_bf[:], ones_d, ones_d.ap)
        xT = kb.sb("xT", [128, 8, T], BF16)
        hv = hT.ap.rearrange("k p t -> p k t")
        for j in range(4):
            load(kb, xT, xT[:, :, j * 1024:(j + 1) * 1024], hT, hv[:, :, j * 1024:(j + 1) * 1024])
        wk = kb.sb("wk", [128, 8, 2048], BF16)
        wq = kb.sb("wq", [128, 8, 1536], BF16)
        wkvv = w_kvs.ap.rearrange("(k p) n -> p k n", p=128)
        wqv = w_q.ap.rearrange("(k p) n -> p k n", p=128)
        for j in range(4):
            load_cast(kb, wk, wk[:, :, j * 512:(j + 1) * 512], w_kvs, wkvv[:, :, j * 512:(j + 1) * 512])
        for j in range(3):
            load_cast(kb, wq, wq[:, :, j * 512:(j + 1) * 512], w_q, wqv[:, :, j * 512:(j + 1) * 512])
        wkv = kb.sb("wkv", [128, 8, 1024], BF16)
        load_cast(kb, wkv, wkv[:], wkv1, wkv1.ap.rearrange("(k p) n -> p k n", p=128))
        memT = kb.sb("memT", [128, 8, 256], BF16)
        kT = kb.sb("kT", [128, 4, 256], BF16)
        vM = kb.sb("vM", [128, 2, 512], BF16)
        xb = [kb.sb("xb%d" % i, [128, 1024], BF16) for i in range(2)]
        ptr = kb.ps("ptr", [128, 1024], BF16)
        psA = [kb.ps("psA%d" % i, [128, 512], F32) for i in range(6)]
        for mc in range(2):
            b_ = xb[mc % 2]
            load_cast(kb, b_, b_[:], mem, mem.ap[mc * 128:(mc + 1) * 128, :])
            tm_to_fm(kb, b_, memT, mc * 128, ident, ptr)
        mem_kv_prep(kb, memT, wkv, kT, vM, psA[0])
        ob = [kb.sb("ob%d" % i, [128, 2048], BF16) for i in range(2)]
        it = 0
        for (wsb, c0, dst) in ((wk, 0, KT), (wq, 0, QT)):
            for h in range(8):
                for half in range(2):
                    o_ = ob[it % 2]
                    it += 1
                    for s in range(4):
                        tsl = slice(half * 2048 + s * 512, half * 2048 + (s + 1) * 512)
                        ps = psA[s % 2]
                        mm_group(kb, ps, ps[:, 0:512], [(wsb[:, k, c0 + h * 128:c0 + (h + 1) * 128], xT[:, k, tsl]) for k in range(8)],
                                 reads=[wsb, xT])
                        if s % 2 == 0:
                            kb.op("act", lambda e, ps=ps, o_=o_, s=s: e.copy(out=o_[:, s * 512:(s + 1) * 512], in_=ps[:, 0:512]),
                                  reads=[ps], writes=[o_])
                        else:
                            kb.op("dve", lambda e, ps=ps, o_=o_, s=s: e.tensor_copy(out=o_[:, s * 512:(s + 1) * 512], in_=ps[:, 0:512]),
                                  reads=[ps], writes=[o_])
                    kb.dma("sp", dst.ap[h, :, half * 2048:(half + 1) * 2048], o_[:], reads=[o_], writes=[dst])
        for tc in range(NT):
            vb = xb[tc % 2]
            for j in range(2):
                ps = psA[j]
                mm_group(kb, ps, ps[:, 0:512], [(xT[:, k, tc * 128:(tc + 1) * 128], wk[:, k, 1024 + j * 512:1024 + (j + 1) * 512]) for k in range(8)],
                         reads=[wk, xT])
                if j == 0:
                    kb.op("act", lambda e, ps=ps, vb=vb: e.copy(out=vb[:, 0:512], in_=ps[:, 0:512]), reads=[ps], writes=[vb])
                else:
                    kb.op("dve", lambda e, ps=ps, vb=vb: e.tensor_copy(out=vb[:, 512:1024], in_=ps[:, 0:512]), reads=[ps], writes=[vb])
            kb.dma("sp", V.ap[tc * 128:(tc + 1) * 128, :], vb[:], reads=[vb], writes=[V])
        mq = kb.sb("mq", [128, 512], BF16)
        pT = kb.sb("pT", [128, 2, 512], BF16)
        rs = kb.sb("rs", [128, 512], F32)
        obm = [kb.sb("obm%d" % i, [128, 512], BF16) for i in range(2)]
        it = 0
        for tt in range(T // 512):
            for h in range(4):
                mm_group(kb, psA[0], psA[0][:, 0:512],
                         [(wq[:, k, 1024 + h * 128:1024 + (h + 1) * 128], xT[:, k, tt * 512:(tt + 1) * 512]) for k in range(8)],
                         reads=[wq, xT])
                mem_attn_tile(kb, h, psA[0], kT, vM, ones_bf, mq, pT, rs, obm[it % 2], [psA[1], psA[2]], psA[3], psA[4],
                              catT, 8 + h, tt * 512)
                it += 1


def alibi_consts():
    pos = np.arange(T)
    hi = (pos // 64) * 64
    lo = pos % 64
    ak = np.zeros((8, 4, T), np.float32)
    aq = np.zeros((8, 4, T), np.float32)
    for h in range(8):
        f = (2.0 ** (-(h + 1))) * 8.0
        ak[h, 0] = f * hi
        ak[h, 1] = f * lo
        ak[h, 2] = 1.0
        ak[h, 3] = 1.0
        aq[h, 0] = 1.0
        aq[h, 1] = 1.0
        aq[h, 2] = -f * hi
        aq[h, 3] = -f * lo
    mtri = np.where(np.arange(128)[:, None] > np.arange(128)[None, :], -32768.0, 0.0).astype(np.float32)
    return ak.astype(ml_dtypes.bfloat16), aq.astype(ml_dtypes.bfloat16), mtri.astype(ml_dtypes.bfloat16)


def phase_diffattn(kb, KT, V, QT, blam, sgd, alk_d, alq_d, mtri_d, ident_d, ones_d, onesf_d, catT):
    SC = 64 ** -0.5
    with kb.scope():
        ones_bf = kb.sb("ones", [128, 128], BF16)
        ones_f = kb.sb("onesf", [128, 128], F32)
        ident = kb.sb("ident", [128, 128], BF16)
        mtri = kb.sb("mtri", [128, 128], BF16)
        load(kb, ones_bf, ones_bf[:], ones_d, ones_d.ap)
        load(kb, ones_f, ones_f[:], onesf_d, onesf_d.ap)
        load(kb, ident, ident[:], ident_d, ident_d.ap)
        load(kb, mtri, mtri[:], mtri_d, mtri_d.ap)
        lv = kb.sb("lv", [128, 256], F32)
        load(kb, lv, lv[:], blam, blam.ap.partition_broadcast(128))
        sm = kb.sb("sm", [128, 8], F32)
        tmp = kb.sb("tmpl", [128, 64], F32)
        load(kb, sm, sm[:, 4:5], sgd, sgd.ap)
        kb.op("dve", lambda e: e.memset(sm[:, 0:4], 0.0), writes=[sm])
        kb.op("dve", lambda e: e.scalar_tensor_tensor(out=tmp[:], in0=lv[:, 0:64], scalar=1.0, in1=lv[:, 64:128], op0=ALU.mult,
                                                      op1=ALU.mult, accum_out=sm[:, 0:1]), reads=[lv, sm], writes=[tmp, sm])
        kb.op("dve", lambda e: e.scalar_tensor_tensor(out=tmp[:], in0=lv[:, 128:192], scalar=1.0, in1=lv[:, 192:256], op0=ALU.mult,
                                                      op1=ALU.mult, accum_out=sm[:, 1:2]), reads=[lv, sm], writes=[tmp, sm])
        kb.op("act", lambda e: e.activation(out=sm[:, 2:4], in_=sm[:, 0:2], func=AF.Exp), reads=[sm], writes=[sm])
        kb.op("dve", lambda e: e.tensor_tensor(out=sm[:, 5:6], in0=sm[:, 3:4], in1=sm[:, 2:3], op=ALU.subtract), reads=[sm], writes=[sm])
        kb.op("dve", lambda e: e.tensor_scalar(out=sm[:, 5:6], in0=sm[:, 5:6], scalar1=-LAM_INIT, scalar2=None, op0=ALU.add),
              reads=[sm], writes=[sm])
        kb.op("dve", lambda e: e.tensor_scalar(out=sm[:, 6:7], in0=sm[:, 4:5], scalar1=(1.0 - LAM_INIT), scalar2=None, op0=ALU.mult),
              reads=[sm], writes=[sm])
        NLAM, GSC = sm[:, 5:6], sm[:, 6:7]
        kth = [kb.sb("kth%d" % i, [128, 2, T], BF16) for i in range(2)]
        qth = [kb.sb("qth%d" % i, [128, 2, T], BF16) for i in range(2)]
        vh = [kb.sb("vh%d" % i, [128, NT, 128], BF16) for i in range(2)]
        NROT = 4
        pb = [kb.sb("pb%d" % i, [128, 512], BF16) for i in range(NROT)]
        psS = [kb.ps("psS%d" % i, [128, 512], F32) for i in range(NROT)]
        psO = [kb.ps("psO%d" % i, [128, 512], F32) for i in range(2)]
        psR = [kb.ps("psR%d" % i, [128, 512], F32) for i in range(2)]
        rs_ = [[kb.sb("rs%d%d" % (f, i), [128, 512], F32) for i in range(2)] for f in range(2)]
        os_ = [[kb.sb("os%d%d" % (f, i), [128, 512], F32) for i in range(2)] for f in range(2)]
        dd = [kb.sb("dd%d" % f, [128, 512], F32) for f in range(2)]
        sq = [kb.sb("sq%d" % f, [128, 512], F32) for f in range(2)]
        obf = [kb.sb("obf%d" % i, [128, 512], BF16) for i in range(2)]
        vv = V.ap.rearrange("(kc p) d -> p kc d", p=128)
        LA = 2
        items = []
        for h in range(8):
            slope = 2.0 ** (-(h + 1))
            for qt in range(T // 512):
                kcs = []
                for kc in range(4 * (qt + 1)):
                    j = kc - 4 * qt
                    if j < 0:
                        min_dist = 512 * qt - (128 * kc + 127)
                        if slope * min_dist > 150.0:
                            continue
                    kcs.append(kc)
                for n_, kc in enumerate(kcs):
                    for c in range(2):
                        items.append((h, qt, kc, c, n_ == 0, n_ == len(kcs) - 1))
        state = {"oi": 0, "rot": 0}
        rots = {}

        def load_head(h):
            k_, q_, v_ = kth[h % 2], qth[h % 2], vh[h % 2]
            for c in range(2):
                load(kb, k_, k_[0:64, c, :], KT, KT.ap[h, c * 64:(c + 1) * 64, :])
                load(kb, q_, q_[0:64, c, :], QT, QT.ap[h, c * 64:(c + 1) * 64, :])
                load(kb, k_, k_[64:68, c, :], alk_d, alk_d.ap[h])
                load(kb, q_, q_[64:68, c, :], alq_d, alq_d.ap[h])
            load(kb, v_, v_[:], V, vv[:, :, h * 128:(h + 1) * 128])

        def stage_a(idx):
            h, qt, kc, c, first, last = items[idx]
            if qt == 0 and first and c == 0:
                load_head(h)
            k_, q_ = kth[h % 2], qth[h % 2]
            j = kc - 4 * qt
            cs = 128 * j if j > 0 else 0
            r = state["rot"] % NROT
            state["rot"] += 1
            rots[idx] = (r, cs)
            ps, p_ = psS[r], pb[r]
            ksl = slice(kc * 128, (kc + 1) * 128)
            qsl = slice(qt * 512 + cs, (qt + 1) * 512)
            if j >= 0:
                kb.op("pe", lambda e: e.matmul(ps[:, cs:512], k_[0:68, c, ksl], q_[0:68, c, qsl], start=True, stop=False),
                      reads=[k_, q_], writes=[ps], inc=False)
                kb.op("pe", lambda e: e.matmul(ps[:, cs:cs + 128], ident[:], mtri[:], start=False, stop=True),
                      reads=[ident, mtri], writes=[ps], inc=True)
            else:
                kb.op("pe", lambda e: e.matmul(ps[:, cs:512], k_[0:68, c, ksl], q_[0:68, c, qsl], start=True, stop=True),
                      reads=[k_, q_], writes=[ps], inc=True)
            kb.op("act", lambda e: e.activation(out=p_[:, cs:512], in_=ps[:, cs:512], func=AF.Exp, scale=SC),
                  reads=[ps], writes=[p_])

        def stage_b(idx):
            h, qt, kc, c, first, last = items[idx]
            v_ = vh[h % 2]
            r, cs = rots.pop(idx)
            p_ = pb[r]
            kb.op("pe", lambda e: e.matmul(psO[c][:, cs:512], v_[:, kc, :], p_[:, cs:512], start=first, stop=last),
                  reads=[v_, p_], writes=[psO[c]], inc=False)
            kb.op("pe", lambda e: e.matmul(psR[c][:, cs:512], ones_bf[:], p_[:, cs:512], start=first, stop=last),
                  reads=[ones_bf, p_], writes=[psR[c]], inc=True)
            if not (last and c == 1):
                return
            qsl = slice(qt * 512, (qt + 1) * 512)
            f = state["oi"] % 2
            state["oi"] += 1
            r0, r1, t0, t1, dd_, sq_, o_ = rs_[f][0], rs_[f][1], os_[f][0], os_[f][1], dd[f], sq[f], obf[f]
            kb.op("act", lambda e: e.copy(out=r0[:], in_=psR[0][:, 0:512]), reads=[psR[0]], writes=[r0])
            kb.op("act", lambda e: e.copy(out=r1[:], in_=psR[1][:, 0:512]), reads=[psR[1]], writes=[r1])
            kb.op("dve", lambda e: e.tensor_tensor(out=t0[:], in0=psO[0][:, 0:512], in1=r1[:], op=ALU.mult), reads=[psO[0], r1], writes=[t0])
            kb.op("dve", lambda e: e.tensor_tensor(out=t1[:], in0=psO[1][:, 0:512], in1=r0[:], op=ALU.mult), reads=[psO[1], r0], writes=[t1])
            kb.op("dve", lambda e: e.scalar_tensor_tensor(out=t0[:], in0=t1[:], scalar=NLAM, in1=t0[:], op0=ALU.mult, op1=ALU.add),
                  reads=[t0, t1, sm], writes=[t0])
            kb.op("pool", lambda e: e.tensor_tensor(out=dd_[:], in0=r0[:], in1=r1[:], op=ALU.mult), reads=[r0, r1], writes=[dd_])
            kb.op("pool", lambda e: e.tensor_tensor(out=sq_[:], in0=t0[:], in1=t0[:], op=ALU.mult), reads=[t0], writes=[sq_])
            kb.op("dve", lambda e: e.scalar_tensor_tensor(out=dd_[:], in0=dd_[:], scalar=EPS, in1=dd_[:], op0=ALU.mult, op1=ALU.mult),
                  reads=[dd_], writes=[dd_])
            r2 = state["rot"] % NROT
            state["rot"] += 1
            ps2 = psS[r2]
            mm_group(kb, ps2, ps2[:, 0:512], [(ones_f[:], sq_[:])], reads=[ones_f, sq_])
            kb.op("dve", lambda e: e.scalar_tensor_tensor(out=dd_[:], in0=ps2[:, 0:512], scalar=1.0 / 128.0, in1=dd_[:], op0=ALU.mult,
                                                          op1=ALU.add), reads=[ps2, dd_], writes=[dd_])
            kb.op("act", lambda e: e.activation(out=dd_[:], in_=dd_[:], func=AF.Ln), reads=[dd_], writes=[dd_])
            kb.op("act", lambda e: e.activation(out=dd_[:], in_=dd_[:], func=AF.Exp, scale=-0.5), reads=[dd_], writes=[dd_])
            kb.op("dve", lambda e: e.tensor_tensor(out=t0[:], in0=t0[:], in1=dd_[:], op=ALU.mult), reads=[t0, dd_], writes=[t0])
            kb.op("dve", lambda e: e.tensor_scalar(out=o_[:], in0=t0[:], scalar1=GSC, scalar2=None, op0=ALU.mult),
                  reads=[t0, sm], writes=[o_])
            kb.dma("sp", catT.ap[h, :, qsl], o_[:], reads=[o_], writes=[catT])

        n = len(items)
        for idx in range(n + LA):
            if idx < n:
                stage_a(idx)
            if idx - LA >= 0:
                stage_b(idx - LA)


def router_factory(wrT_d, identf_d, tri_d, onesf_d, eoff_d, xbuf, rt_i, rt_g):
    def factory(kb):
        wrs = kb.sb("wrs", [128, 8, NE], F32)
        load(kb, wrs, wrs[:], wrT_d, wrT_d.ap.rearrange("(k p) e -> p k e", p=128))
        identf = kb.sb("identf", [128, 128], F32)
        load(kb, identf, identf[:], identf_d, identf_d.ap)
        xT32 = [kb.sb("xT32_%d" % i, [128, 8, 128], F32) for i in range(2)]
        psT = [kb.ps("psT%d" % i, [128, 512], F32) for i in range(2)]
        tri = kb.sb("tri", [128, 128], F32)
        onesf = kb.sb("onesf", [128, 128], F32)
        eoff = kb.sb("eoff", [128, NE], F32)
        load(kb, tri, tri[:], tri_d, tri_d.ap)
        load(kb, onesf, onesf[:], onesf_d, onesf_d.ap)
        load(kb, eoff, eoff[:], eoff_d, eoff_d.ap)
        ND = 8
        many = lambda nm, shp, dt=F32, n=ND: [kb.sb("%s%d" % (nm, i), shp, dt) for i in range(n)]
        lg, mx, s1, s2, sel = many("lg", [128, 8]), many("mx", [128, 8]), many("s1", [128, 8]), many("s2", [128, 8]), many("sel", [128, 8])
        gg, hb = many("gg", [128, 4]), many("hbx", [128, D], BF16, 6)
        dia, dib = many("dia", [128, 1], I32), many("dib", [128, 1], I32)
        dest = kb.sb("dest", [128, 8], F32)
        tmp8 = kb.sb("tmp8", [128, 8], F32)
        base = kb.sb("base", [128, 8], F32)
        df = kb.sb("df", [128, 2], F32)
        psPt = kb.ps("psP", [128, 512], F32)
        slots = [Buf("psPs%d" % i, psPt.t, "psum") for i in range(16)]

        def sl(i, a, b):
            q = i % 16
            return slots[q], psPt.t[:, 32 * q + a:32 * q + b]
        kb.op("dve", lambda e: e.memset(base[:], 0.0), writes=[base])

        def h0(xn, row0, i):
            x32 = xT32[i % 2]
            for half in range(2):
                pt = psT[half]
                for k4 in range(4):
                    k = half * 4 + k4
                    kb.op("pe", lambda e, k=k, k4=k4, pt=pt: e.transpose(out=pt[:, k4 * 128:(k4 + 1) * 128], in_=xn[:, k * 128:(k + 1) * 128],
                                                                       identity=identf[:]),
                          reads=[xn, identf], writes=[pt], inc=(k4 == 3))
            kb.op("act", lambda e: e.copy(out=hb[i % 6][:], in_=xn[:]), reads=[xn], writes=[hb[i % 6]])
            kb.op("act", lambda e: e.copy(out=x32[:, 0:4, :], in_=psT[0][:, 0:512].rearrange("p (k t) -> p k t", k=4)),
                  reads=[psT[0]], writes=[x32])
            kb.op("dve", lambda e: e.tensor_copy(out=x32[:, 4:8, :], in_=psT[1][:, 0:512].rearrange("p (k t) -> p k t", k=4)),
                  reads=[psT[1]], writes=[x32])

        def h1(xn, row0, i):
            x32 = xT32[i % 2]
            sb_, ap_ = sl(i, 16, 24)
            for k in range(8):
                kb.op("pe", lambda e, k=k: e.matmul(ap_, x32[:, k, :], wrs[:, k, :], start=(k == 0), stop=(k == 7)),
                      reads=[x32, wrs], writes=[sb_], inc=(k == 7))

        def h2(xn, row0, i):
            p = i % ND
            lg_, mx_, s1_, s2_, sel_, gg_ = lg[p], mx[p], s1[p], s2[p], sel[p], gg[p]
            sb_, ap_ = sl(i, 16, 24)
            kb.op("dve", lambda e: e.tensor_copy(out=lg_[:], in_=ap_), reads=[sb_], writes=[lg_])
            kb.op("dve", lambda e: e.max(out=mx_[:], in_=lg_[:]), reads=[lg_], writes=[mx_])
            kb.op("dve", lambda e: e.tensor_scalar(out=s1_[:], in0=lg_[:], scalar1=mx_[:, 0:1], scalar2=None, op0=ALU.is_ge),
                  reads=[lg_, mx_], writes=[s1_])
            kb.op("dve", lambda e: e.tensor_scalar(out=sel_[:], in0=lg_[:], scalar1=mx_[:, 1:2], scalar2=None, op0=ALU.is_ge),
                  reads=[lg_, mx_], writes=[sel_])
            kb.op("dve", lambda e: e.tensor_tensor(out=s2_[:], in0=sel_[:], in1=s1_[:], op=ALU.subtract), reads=[sel_, s1_], writes=[s2_])
            kb.op("dve", lambda e: e.tensor_tensor(out=gg_[:, 2:3], in0=mx_[:, 1:2], in1=mx_[:, 0:1], op=ALU.subtract), reads=[mx_],
                  writes=[gg_])

        def h3(xn, row0, i):
            p = i % ND
            sel_, gg_ = sel[p], gg[p]
            sb_, ap_ = sl(i, 0, 16)
            kb.op("pe", lambda e: e.matmul(ap_[:, 0:8], tri[:], sel_[:], start=True, stop=True), reads=[tri, sel_], writes=[sb_], inc=False)
            kb.op("pe", lambda e: e.matmul(ap_[:, 8:16], onesf[:], sel_[:], start=True, stop=True), reads=[onesf, sel_], writes=[sb_],
                  inc=True)
            kb.op("act", lambda e: e.activation(out=gg_[:, 3:4], in_=gg_[:, 2:3], func=AF.Exp), reads=[gg_], writes=[gg_])

        def h4(xn, row0, i):
            p = i % ND
            s1_, s2_, gg_, hb_, da_, db_ = s1[p], s2[p], gg[p], hb[i % 6], dia[p], dib[p]
            sb_, ap_ = sl(i, 0, 16)
            kb.op("dve", lambda e: e.tensor_tensor(out=dest[:], in0=ap_[:, 0:8], in1=base[:], op=ALU.add), reads=[sb_, base], writes=[dest])
            kb.op("dve", lambda e: e.tensor_scalar(out=tmp8[:], in0=dest[:], scalar1=float(CAP), scalar2=1.0e6, op0=ALU.is_ge, op1=ALU.mult),
                  reads=[dest], writes=[tmp8])
            kb.op("dve", lambda e: e.tensor_tensor(out=dest[:], in0=dest[:], in1=tmp8[:], op=ALU.add), reads=[dest, tmp8], writes=[dest])
            kb.op("dve", lambda e: e.tensor_tensor(out=dest[:], in0=dest[:], in1=eoff[:], op=ALU.add), reads=[dest, eoff], writes=[dest])
            kb.op("dve", lambda e: e.tensor_tensor(out=base[:], in0=base[:], in1=ap_[:, 8:16], op=ALU.add), reads=[sb_, base], writes=[base])
            kb.op("dve", lambda e: e.memset(df[:], 0.0), writes=[df])
            kb.op("dve", lambda e: e.scalar_tensor_tensor(out=tmp8[:], in0=s1_[:], scalar=1.0, in1=dest[:], op0=ALU.mult, op1=ALU.mult,
                                                          accum_out=df[:, 0:1]), reads=[s1_, dest, df], writes=[tmp8, df])
            kb.op("dve", lambda e: e.scalar_tensor_tensor(out=tmp8[:], in0=s2_[:], scalar=1.0, in1=dest[:], op0=ALU.mult, op1=ALU.mult,
                                                          accum_out=df[:, 1:2]), reads=[s2_, dest, df], writes=[tmp8, df])
            kb.op("dve", lambda e: e.tensor_copy(out=da_[:], in_=df[:, 0:1]), reads=[df], writes=[da_])
            kb.op("dve", lambda e: e.tensor_copy(out=db_[:], in_=df[:, 1:2]), reads=[df], writes=[db_])
            kb.op("dve", lambda e: e.tensor_scalar(out=gg_[:, 0:1], in0=gg_[:, 3:4], scalar1=1.0, scalar2=None, op0=ALU.add), reads=[gg_],
                  writes=[gg_])
            kb.op("dve", lambda e: e.reciprocal(out=gg_[:, 0:1], in_=gg_[:, 0:1]), reads=[gg_], writes=[gg_])
            kb.op("dve", lambda e: e.tensor_tensor(out=gg_[:, 1:2], in0=gg_[:, 3:4], in1=gg_[:, 0:1], op=ALU.mult), reads=[gg_], writes=[gg_])
            for j, d_ in enumerate((da_, db_)):
                kb.idma(xbuf.ap[:, :], bass.IndirectOffsetOnAxis(ap=d_[:, 0:1], axis=0), hb_[:], None, NE * CAP - 1,
                        reads=[hb_, d_], writes=[], semb=hb_)
                kb.dma("sp", rt_i.ap[j, row0:row0 + 128, :], d_[:], reads=[d_], writes=[rt_i])
            kb.dma("sp", rt_g.ap[row0:row0 + 128, :], gg_[:, 0:2], reads=[gg_], writes=[rt_g])
        return [h0, h1, h2, h3, h4]
    return factory


def phase_moe(kb, xbuf, moe_w13, moe_w2, ident_d, ybuf):
    NS = CAP // 128
    with kb.scope():
        ident = kb.sb("ident", [128, 128], BF16)
        load(kb, ident, ident[:], ident_d, ident_d.ap)
        FS = FFNState(kb, CAP)
        w2s = kb.sb("w2s", [128, 28, D], BF16)
        xsT = kb.sb("xsT", [128, 8, CAP], BF16)
        xb = [kb.sb("xb%d" % i, [128, D], BF16) for i in range(2)]
        yrow = [kb.sb("yrow%d" % i, [128, D], F32) for i in range(2)]
        ptr = kb.ps("ptr", [128, 1024], BF16)
        psY = [kb.ps("psY%d" % j, [128, 512], F32) for j in range(2)]
        pre = None
        yi = 0
        for ex in range(NE):
            w13v = moe_w13.ap[ex].rearrange("(k p) n -> p k n", p=128)
            w2v = moe_w2.ap[ex].rearrange("(k p) n -> p k n", p=128)
            for sl in range(NS):
                b_ = xb[sl % 2]
                load(kb, b_, b_[:], xbuf, xbuf.ap[ex * CAP + sl * 128:ex * CAP + (sl + 1) * 128, :])
                tm_to_fm(kb, b_, xsT, sl * 128, ident, ptr, ev=("act" if sl % 2 == 0 else "dve"))
            for j in range(7):
                load_cast(kb, w2s, w2s[:, j * 4:(j + 1) * 4, :], moe_w2, w2v[:, j * 4:(j + 1) * 4, :])
            nxt = []

            def prefetch(ex=ex, nxt=nxt):
                if ex + 1 < NE:
                    nv = moe_w13.ap[ex + 1].rearrange("(k p) n -> p k n", p=128)
                    nxt.append(w13_load(kb, FS, moe_w13, nv, 0))
            swiglu_stage1(kb, FS, xsT, CAP, moe_w13, w13v, first_blocks=pre, next_prefetch=prefetch)
            pre = nxt if nxt else None
            for sub in range(NS):
                yr = yrow[yi % 2]
                yi += 1
                for j in range(2):
                    mm_group(kb, psY[j], psY[j][:, 0:512],
                             [(FS.act[:, f, sub * 128:(sub + 1) * 128], w2s[:, f, j * 512:(j + 1) * 512]) for f in range(28)],
                             reads=[FS.act, w2s])
                kb.op("act", lambda e, yr=yr: e.copy(out=yr[:, 0:512], in_=psY[0][:, 0:512]), reads=[psY[0]], writes=[yr])
                kb.op("dve", lambda e, yr=yr: e.tensor_copy(out=yr[:, 512:1024], in_=psY[1][:, 0:512]), reads=[psY[1]], writes=[yr])
                r0 = ex * CAP + sub * 128
                kb.dma("sp", ybuf.ap[r0:r0 + 128, :], yr[:], reads=[yr], writes=[ybuf])


def phase_combine(kb, ybuf, rt_i, rt_g, resid, ln_g, ln_b, ident_d, out):
    with kb.scope():
        P = LNPipe(kb, ln_g, ln_b, ident_d, resid, out, transposes=False)
        dia = [kb.sb("dia%d" % i, [128, 1], I32) for i in range(3)]
        dib = [kb.sb("dib%d" % i, [128, 1], I32) for i in range(3)]
        gg = [kb.sb("gg%d" % i, [128, 2], F32) for i in range(3)]
        y1 = [kb.sb("y1_%d" % i, [128, D], F32) for i in range(3)]
        y2 = [kb.sb("y2_%d" % i, [128, D], F32) for i in range(3)]
        for i in range(3):
            kb.op("dve", lambda e, i=i: e.memset(y1[i][:], 0.0), writes=[y1[i]])
            kb.op("dve", lambda e, i=i: e.memset(y2[i][:], 0.0), writes=[y2[i]])

        def gather(tc):
            da_, db_, g_, a_, b_ = dia[tc % 3], dib[tc % 3], gg[tc % 3], y1[tc % 3], y2[tc % 3]
            load(kb, da_, da_[:], rt_i, rt_i.ap[0, tc * 128:(tc + 1) * 128, :])
            load(kb, db_, db_[:], rt_i, rt_i.ap[1, tc * 128:(tc + 1) * 128, :])
            load(kb, g_, g_[:], rt_g, rt_g.ap[tc * 128:(tc + 1) * 128, :])
            kb.idma(a_[:], None, ybuf.ap[:, :], bass.IndirectOffsetOnAxis(ap=da_[:, 0:1], axis=0), NE * CAP - 1,
                    reads=[ybuf, da_], writes=[a_], semb=a_)
            kb.idma(b_[:], None, ybuf.ap[:, :], bass.IndirectOffsetOnAxis(ap=db_[:, 0:1], axis=0), NE * CAP - 1,
                    reads=[ybuf, db_], writes=[b_], semb=b_)
        gather(0)
        gather(1)
        P.prefetch(0)
        for tc in range(NT):
            if tc + 2 < NT:
                gather(tc + 2)
            if tc + 1 < NT:
                P.prefetch((tc + 1) * 128)
            g_, a_, b_ = gg[tc % 3], y1[tc % 3], y2[tc % 3]
            kb.op("act", lambda e, a_=a_, g_=g_: e.activation(out=a_[:], in_=a_[:], func=AF.Copy, scale=g_[:, 0:1]),
                  reads=[a_, g_], writes=[a_])
            kb.op("dve", lambda e, a_=a_, b_=b_, g_=g_: e.scalar_tensor_tensor(out=a_[:], in0=b_[:], scalar=g_[:, 1:2], in1=a_[:],
                                                                               op0=ALU.mult, op1=ALU.add),
                  reads=[a_, b_, g_], writes=[a_])
            P.push([(a_, a_[:, 0:512]), (a_, a_[:, 512:1024])], tc * 128)
        P.flush()


def build_program():
    kb = KB()
    EI = "ExternalInput"
    d = {}
    d["x"] = kb.dram("x", [T, D], F32, EI)
    d["mem"] = kb.dram("mem", [256, D], F32, EI)
    d["a_w_in"] = kb.dram("a_w_in", [D, 2560], F32, EI)
    d["a_w_rgate"] = kb.dram("a_w_rgate", [8, 128, 128], F32, EI)
    d["a_w_igate"] = kb.dram("a_w_igate", [8, 128, 128], F32, EI)
    d["mem_w_kv0"] = kb.dram("mem_w_kv0", [D, 1024], F32, EI)
    d["mem_w_kv1"] = kb.dram("mem_w_kv1", [D, 1024], F32, EI)
    d["pp1"] = kb.dram("pp1", [128, 80], F32, EI)
    d["a_w_out"] = kb.dram("a_w_out", [1536, D], F32, EI)
    d["b_w_out"] = kb.dram("b_w_out", [1536, D], F32, EI)
    d["ffn_w13"] = kb.dram("ffn_w13", [D, 2 * FFN], F32, EI)
    d["ffn_w2"] = kb.dram("ffn_w2", [FFN, D], F32, EI)
    d["w_kv_shared"] = kb.dram("w_kv_shared", [D, 2048], F32, EI)
    d["b_w_q"] = kb.dram("b_w_q", [D, 1536], F32, EI)
    d["blam"] = kb.dram("blam", [256], F32, EI)
    d["sgd"] = kb.dram("sgd", [128, 1], F32, EI)
    d["wrT"] = kb.dram("wrT", [D, NE], F32, EI)
    d["ident_f"] = kb.dram("ident_f", [128, 128], F32, EI)
    d["moe_w13"] = kb.dram("moe_w13", [NE, D, 2 * FFN], F32, EI)
    d["moe_w2"] = kb.dram("moe_w2", [NE, FFN, D], F32, EI)
    for l in range(2):
        for s_ in range(2):
            d["lg%d%d" % (l, s_)] = kb.dram("lg%d%d" % (l, s_), [D], F32, EI)
            d["lb%d%d" % (l, s_)] = kb.dram("lb%d%d" % (l, s_), [D], F32, EI)
    d["ident"] = kb.dram("ident", [128, 128], BF16, EI)
    d["ones_bf"] = kb.dram("ones_bf", [128, 128], BF16, EI)
    d["ones_f"] = kb.dram("ones_f", [128, 128], F32, EI)
    d["tri"] = kb.dram("tri", [128, 128], F32, EI)
    d["eoff"] = kb.dram("eoff", [128, NE], F32, EI)
    d["alk"] = kb.dram("alk", [8, 4, T], BF16, EI)
    d["alq"] = kb.dram("alq", [8, 4, T], BF16, EI)
    d["mtri"] = kb.dram("mtri", [128, 128], BF16, EI)
    out = kb.dram("out", [T, D], F32, "ExternalOutput")
    catT0 = kb.dram("catT0", [12, 128, T], BF16)
    h1 = kb.dram("h1", [T, D], F32)
    h1T = kb.dram("h1T", [8, 128, T], BF16)
    h2 = kb.dram("h2", [T, D], F32)
    h2T = kb.dram("h2T", [8, 128, T], BF16)
    KT = kb.dram("KT", [8, 128, T], BF16)
    QT = kb.dram("QT", [8, 128, T], BF16)
    V = kb.dram("V", [T, 1024], BF16)
    catT1 = kb.dram("catT1", [12, 128, T], BF16)
    h3 = kb.dram("h3", [T, D], F32)
    xbuf = kb.dram("xbuf", [NE * CAP, D], BF16)
    ybuf = kb.dram("ybuf", [NE * CAP, D], F32)
    rt_i = kb.dram("rt_i", [2, T, 1], I32)
    rt_g = kb.dram("rt_g", [T, 2], F32)

    phase1(kb, d["x"], d["mem"], d["a_w_in"], d["a_w_rgate"], d["a_w_igate"], d["mem_w_kv0"], d["pp1"], d["ident"], d["ones_bf"], catT0)
    phase_outproj(kb, catT0, d["a_w_out"], d["x"], d["lg00"], d["lb00"], d["ident"], h1, h1T)
    phase_ffn(kb, h1T, h1, d["ffn_w13"], d["ffn_w2"], d["lg01"], d["lb01"], d["ident"], h2, h2T)
    phase_qkv(kb, h2T, d["mem"], d["w_kv_shared"], d["b_w_q"], d["mem_w_kv1"], d["ident"], d["ones_bf"], KT, V, QT, catT1, xbuf=xbuf)
    phase_diffattn(kb, KT, V, QT, d["blam"], d["sgd"], d["alk"], d["alq"], d["mtri"], d["ident"], d["ones_bf"], d["ones_f"], catT1)
    phase_outproj(kb, catT1, d["b_w_out"], h2, d["lg10"], d["lb10"], d["ident"], h3, None,
                  hooks_factory=router_factory(d["wrT"], d["ident_f"], d["tri"], d["ones_f"], d["eoff"], xbuf, rt_i, rt_g))
    phase_moe(kb, xbuf, d["moe_w13"], d["moe_w2"], d["ident"], ybuf)
    phase_combine(kb, ybuf, rt_i, rt_g, h3, d["lg11"], d["lb11"], d["ident"], out)
    kb.finish()
    return kb


def host_inputs(inp):
    f = lambda a: np.ascontiguousarray(np.asarray(a, dtype=np.float32))
    hc = host_consts()
    alk, alq, mtri = alibi_consts()
    pp = np.zeros((128, 80), np.float32)
    cw = np.asarray(inp["a_conv_w"], np.float32)[0]
    for c in range(8):
        for j in range(4):
            pp[:, c * 4 + j] = cw[j, c * 128:(c + 1) * 128]
    pp[:, 32:40] = pack_cols(inp["a_conv_b"][0], 8)
    pp[:, 40:48] = pack_cols(inp["a_b_rgate"][0], 8)
    pp[:, 48:56] = pack_cols(inp["a_b_igate"][0], 8)
    pp[:, 56:64] = pack_cols(inp["a_lambda"][0], 8)
    shared = {
        "a_w_in": f(inp["a_w_in"][0]), "a_w_rgate": f(inp["a_w_rgate"][0]), "a_w_igate": f(inp["a_w_igate"][0]),
        "mem_w_kv0": f(inp["mem_w_kv"][0]), "mem_w_kv1": f(inp["mem_w_kv"][1]), "pp1": pp,
        "a_w_out": f(inp["a_w_out"][0]), "b_w_out": f(inp["b_w_out"][0]),
        "ffn_w13": f(inp["ffn_w13"][0]), "ffn_w2": f(inp["ffn_w2"][0]),
        "w_kv_shared": f(inp["w_kv_shared"]), "b_w_q": f(inp["b_w_q"][0]),
        "blam": f(np.asarray(inp["b_lambda"])[0].reshape(256)), "sgd": f(np.asarray(inp["b_subln_g"])[0].reshape(128, 1)),
        "wrT": f(np.asarray(inp["moe_router"])[0]), "ident_f": np.eye(128, dtype=np.float32),
        "moe_w13": f(inp["moe_w13"][0]), "moe_w2": f(inp["moe_w2"][0]),
        "ident": hc["ident"], "ones_bf": hc["ones_bf"], "ones_f": hc["ones_f"], "tri": hc["tri"], "eoff": hc["eoff"],
        "alk": alk, "alq": alq, "mtri": mtri,
    }
    for l in range(2):
        for s_ in range(2):
            shared["lg%d%d" % (l, s_)] = f(inp["ln_g"][l, s_])
            shared["lb%d%d" % (l, s_)] = f(inp["ln_b"][l, s_])
    maps = []
    for b in range(8):
        m = dict(shared)
        m["x"] = f(inp["x"][b])
        m["mem"] = f(inp["mem"][b])
        maps.append(m)
    return maps


_PROG = None


def kernel(**inputs):
    global _PROG
    if _PROG is None:
        _PROG = build_program()
    maps = host_inputs(inputs)
    res = run_bass_kernel_spmd(_PROG.nc, maps, core_ids=list(range(8)))
    return np.stack([np.asarray(r["out"], dtype=np.float32) for r in res.results], axis=0)
```
